# Optimizing a Trainium2 kernel written in Bass

```python
import jax, jax.numpy as jnp
from jax import lax
import numpy as np

D_MODEL = 4096
BATCH = 2
SEQ = 8192
DEPTH = 2

N_A_LAYERS = DEPTH // 2
N_B_LAYERS = DEPTH - N_A_LAYERS
N_DENSE = (DEPTH + 1) // 2
N_MOE = DEPTH // 2
CONV_WIDTH = 3
HEAD_DIM = 128
N_HEADS = D_MODEL // HEAD_DIM
D_FF = 11008
N_EXPERTS = 8
TOP_K = 2
D_EXPERT = D_MODEL
PLE_DIM = 256
Q_BLOCK = 128
EPS = 1e-6

kernel_name = 'yoco_shortconv_stickbreaking_moe_trunk'


def rmsnorm(x, g):
    x32 = x.astype(jnp.float32)
    y = x32 * lax.rsqrt(jnp.mean(x32 * x32, axis=-1, keepdims=True) + EPS)
    return (y * g.astype(jnp.float32)).astype(x.dtype)


def to_heads(t):
    b, s, _ = t.shape
    return t.reshape(b, s, N_HEADS, HEAD_DIM).transpose(0, 2, 1, 3)


def from_heads(o):
    b, h, s, dh = o.shape
    return o.transpose(0, 2, 1, 3).reshape(b, s, h * dh)


def short_conv_mixer(h, w_in, conv_w, w_out):
    s = h.shape[1]
    bcx = h @ w_in
    b_gate, c_gate, u = jnp.split(bcx, 3, axis=-1)
    u = c_gate * u
    u_pad = jnp.pad(u, ((0, 0), (CONV_WIDTH - 1, 0), (0, 0)))
    y = conv_w[0] * u_pad[:, 0:s]
    for tap in range(1, CONV_WIDTH):
        y = y + conv_w[tap] * u_pad[:, tap:tap + s]
    return (b_gate * y) @ w_out


def stick_breaking_attention(q, k, v):
    b, h, s, dh = q.shape
    n_blocks = s // Q_BLOCK
    qb = q.reshape(b, h, n_blocks, Q_BLOCK, dh).transpose(2, 0, 1, 3, 4)
    starts = jnp.arange(n_blocks, dtype=jnp.int32) * Q_BLOCK
    key_pos = jnp.arange(s, dtype=jnp.int32)
    scale = HEAD_DIM ** -0.5

    def one_block(args):
        q_blk, start = args
        z = jnp.einsum('bhqd,bhkd->bhqk', q_blk, k, preferred_element_type=jnp.float32) * scale
        q_pos = start + jnp.arange(Q_BLOCK, dtype=jnp.int32)
        strict = key_pos[None, :] < q_pos[:, None]
        log_beta = jax.nn.log_sigmoid(z)
        log_keep = jnp.where(strict, log_beta - z, 0.0)
        between = lax.cumsum(log_keep, axis=3, reverse=True) - log_keep
        w = jnp.where(strict, jnp.exp(log_beta + between), 0.0)
        return jnp.einsum('bhqk,bhkd->bhqd', w.astype(v.dtype), v)

    out = lax.map(one_block, (qb, starts))
    return out.transpose(1, 2, 0, 3, 4).reshape(b, h, s, dh)


def swiglu(h, w_gu, w_down):
    g, u = jnp.split(h @ w_gu, 2, axis=-1)
    return (jax.nn.silu(g) * u) @ w_down


def moe_swiglu(h, w_router, b_router, w_gu, w_down):
    b, s, d = h.shape
    t = h.reshape(b * s, d)
    logits = jnp.dot(t, w_router, preferred_element_type=jnp.float32) + b_router.astype(jnp.float32)
    top_logits, top_idx = lax.top_k(logits, TOP_K)
    top_w = jax.nn.softmax(top_logits, axis=-1)
    gates = jnp.sum(jax.nn.one_hot(top_idx, N_EXPERTS, dtype=jnp.float32) * top_w[..., None], axis=1)
    gates = gates.astype(h.dtype)
    y = gates[:, 0:1] * swiglu(t, w_gu[0], w_down[0])
    for e in range(1, N_EXPERTS):
        y = y + gates[:, e:e + 1] * swiglu(t, w_gu[e], w_down[e])
    return y.reshape(b, s, d)


def setup_inputs(seed: int = 0) -> dict:
    key = jax.random.key(seed)
    ks = iter(jax.random.split(key, 32))
    f32 = jnp.float32
    out_scale = (2.0 * DEPTH) ** -0.5

    def w(shape, fan_in, extra=1.0):
        return jax.random.normal(next(ks), shape, f32) * (fan_in ** -0.5) * extra

    def gain(shape):
        return 1.0 + 0.02 * jax.random.normal(next(ks), shape, f32)

    d = D_MODEL
    return {
        'x': jax.random.normal(next(ks), (BATCH, SEQ, d), f32),
        'p': jax.random.normal(next(ks), (DEPTH, BATCH, SEQ, PLE_DIM), f32),
        'a_norm': gain((N_A_LAYERS, d)),
        'a_w_in': w((N_A_LAYERS, d, 3 * d), d),
        'a_conv_w': w((N_A_LAYERS, CONV_WIDTH, d), CONV_WIDTH),
        'a_w_out': w((N_A_LAYERS, d, d), d, out_scale),
        'kv_norm': gain((d,)),
        'w_kv': w((d, 2 * N_HEADS * HEAD_DIM), d),
        'b_norm': gain((N_B_LAYERS, d)),
        'b_w_q': w((N_B_LAYERS, d, N_HEADS * HEAD_DIM), d),
        'b_w_o': w((N_B_LAYERS, N_HEADS * HEAD_DIM, d), N_HEADS * HEAD_DIM, out_scale),
        'ffn_norm': gain((DEPTH, d)),
        'dense_w_gu': w((N_DENSE, d, 2 * D_FF), d),
        'dense_w_down': w((N_DENSE, D_FF, d), D_FF, out_scale),
        'moe_w_router': w((N_MOE, d, N_EXPERTS), d),
        'moe_b_router': 0.01 * jax.random.normal(next(ks), (N_MOE, N_EXPERTS), f32),
        'moe_w_gu': w((N_MOE, N_EXPERTS, d, 2 * D_EXPERT), d),
        'moe_w_down': w((N_MOE, N_EXPERTS, D_EXPERT, d), D_EXPERT, out_scale),
        'ple_norm': gain((DEPTH, d)),
        'ple_w_up': w((DEPTH, PLE_DIM, d), PLE_DIM, out_scale),
        'ple_w_gate': w((DEPTH, d, d), d),
        'final_norm': gain((d,)),
    }


def reference(x, p, a_norm, a_w_in, a_conv_w, a_w_out, kv_norm, w_kv, b_norm, b_w_q, b_w_o,
              ffn_norm, dense_w_gu, dense_w_down, moe_w_router, moe_b_router, moe_w_gu, moe_w_down,
              ple_norm, ple_w_up, ple_w_gate, final_norm):
    h = x
    k = None
    v = None
    for i in range(DEPTH):
        if i < N_A_LAYERS:
            h = h + short_conv_mixer(rmsnorm(h, a_norm[i]), a_w_in[i], a_conv_w[i], a_w_out[i])
        else:
            j = i - N_A_LAYERS
            if j == 0:
                k_flat, v_flat = jnp.split(rmsnorm(h, kv_norm) @ w_kv, 2, axis=-1)
                k = to_heads(k_flat)
                v = to_heads(v_flat)
            q = to_heads(rmsnorm(h, b_norm[j]) @ b_w_q[j])
            h = h + from_heads(stick_breaking_attention(q, k, v)) @ b_w_o[j]
        hn = rmsnorm(h, ffn_norm[i])
        if i % 2 == 0:
            h = h + swiglu(hn, dense_w_gu[i // 2], dense_w_down[i // 2])
        else:
            m = i // 2
            h = h + moe_swiglu(hn, moe_w_router[m], moe_b_router[m], moe_w_gu[m], moe_w_down[m])
        gate = jax.nn.sigmoid(rmsnorm(h, ple_norm[i]) @ ple_w_gate[i])
        h = h + (p[i] @ ple_w_up[i]) * gate
    return rmsnorm(h, final_norm)
```

```python
import numpy as np
from contextlib import ExitStack
import concourse.bass as bass
import concourse.mybir as mybir
from concourse.bass_utils import run_bass_kernel_spmd

F32 = mybir.dt.float32
BF16 = mybir.dt.bfloat16
AF = mybir.ActivationFunctionType
ALU = mybir.AluOpType
AX = mybir.AxisListType

D = 4096
DC = 32
T = 2048
NTG = 4
TG = 512
DFF = 11008
FC = 86
NE = 8
PLE = 256
EPS = 1e-6
NSLOT = 4
ENGS = ["tensor", "vector", "scalar", "gpsimd", "sync"]

V_ANORM, V_CW0, V_CW1, V_CW2, V_FFN0, V_PLE0, V_KV, V_BN, V_FFN1, V_PLE1, V_FIN = [32 * i for i in range(11)]
NV = 32 * 11


class Event:
    __slots__ = ("sem", "value", "eng")

    def __init__(self, eng):
        self.sem = None
        self.value = None
        self.eng = eng


class Sched:
    def __init__(self, nc, st):
        self.nc = nc
        self.st = st
        self.sems = {}
        self.ops = {e: [] for e in ENGS}
        self.eng_cnt = {e: 0 for e in ENGS}
        self.pending = {e: [] for e in ENGS}
        self.last_sig = {e: None for e in ENGS}
        self.dma_last = {}
        self.last_w = {}
        self.readers = {}
        self.n_dsem = 0

    def sem(self, name):
        if name not in self.sems:
            self.sems[name] = self.st.enter_context(self.nc.semaphore(name))
        return self.sems[name]

    def new_dma_sem(self):
        self.n_dsem += 1
        return "dx%d" % self.n_dsem

    def op(self, eng, fn, reads=(), writes=(), dma=None, signal=True):
        waits = []
        for r in reads:
            ev = self.last_w.get(r)
            if ev is not None:
                waits.append(ev)
        for w in writes:
            ev = self.last_w.get(w)
            if ev is not None:
                waits.append(ev)
            waits.extend(self.readers.get(w, ()))
        ev = Event(eng)
        inc = None
        if dma is not None:
            prev = self.dma_last.get(dma)
            c = (prev.value if prev is not None else 0) + 16
            ev.sem, ev.value = dma, c
            self.dma_last[dma] = ev
            inc = (dma, 16)
        elif signal:
            self.eng_cnt[eng] += 1
            ev.sem, ev.value = "e_" + eng, self.eng_cnt[eng]
            for p in self.pending[eng]:
                p.sem, p.value = ev.sem, ev.value
            self.pending[eng] = []
            self.last_sig[eng] = ev
            inc = (ev.sem, 1)
        else:
            self.pending[eng].append(ev)
        seen = []
        for w in waits:
            if w is ev:
                continue
            if not any(w is s for s in seen):
                seen.append(w)
        self.ops[eng].append((seen, fn, inc))
        for r in reads:
            self.readers.setdefault(r, []).append(ev)
        for w in writes:
            self.last_w[w] = ev
            self.readers[w] = []
        return ev

    def wait_all(self, eng, events):
        self.ops[eng].append((list(events), None, None))

    def barrier(self):
        evs = [e for e in self.last_sig.values() if e is not None] + list(self.dma_last.values())
        for eng in ENGS:
            assert not self.pending[eng], "barrier with pending events on " + eng
            self.wait_all(eng, evs)
        self.last_w = {}
        self.readers = {}

    def emit(self, block):
        S = self
        for e in ENGS:
            S.sem("e_" + e)

        def run(eng_name):
            def body(e):
                waited = {}
                for waits, fn, inc in S.ops[eng_name]:
                    need = {}
                    for w in waits:
                        if w.sem is None:
                            raise RuntimeError("unresolved event (engine %s)" % w.eng)
                        if eng_name == "tensor" and w.sem == "e_tensor":
                            continue
                        if w.value > waited.get(w.sem, 0):
                            need[w.sem] = max(need.get(w.sem, 0), w.value)
                    for s, v in need.items():
                        e.wait_ge(S.sem(s), v)
                        waited[s] = v
                    if fn is None:
                        continue
                    ins = fn(e)
                    if inc is not None:
                        ins.then_inc(S.sem(inc[0]), inc[1])
            return body

        for eng in ENGS:
            for waits, fn, inc in S.ops[eng]:
                if inc is not None:
                    S.sem(inc[0])
        block.tensor(run("tensor"))
        block.vector(run("vector"))
        block.scalar(run("scalar"))
        block.gpsimd(run("gpsimd"))
        block.sync(run("sync"))


class Builder:
    def __init__(self, nc, st):
        self.nc = nc
        self.st = st
        self.S = Sched(nc, st)
        sb = lambda n, shape, dt: st.enter_context(nc.sbuf_tensor("s_" + n, shape, dt))
        self.act = sb("act", [128, DC * T], BF16)
        self.wsl = sb("wsl", [128, NSLOT, 4096], BF16)
        self.xar = sb("xar", [128, 4096], F32)
        self.rstd = sb("rstd", [128, T + 8], F32)
        self.ft = sb("ft", [128, 4, TG], F32)
        self.ht = sb("ht", [128, 4, TG], F32)
        self.bt = sb("bt", [128, 4, TG], BF16)
        self.vecs = sb("vecs", [128, NV], F32)
        self.ones = sb("ones", [128, 128], BF16)
        self.small = sb("small", [128, 64], F32)
        self.ps = st.enter_context(nc.psum_tensor("ps", [128, 8, TG], F32))
        self.fi = 0
        self.bi = 0
        self.hi = 0
        self.act3 = self.act[:, :].rearrange("p (c t) -> p c t", t=T)
        self.xar_bf = self.xar.bitcast(BF16)

    def ftile(self):
        i = self.fi % 4
        self.fi += 1
        return i

    def htile(self):
        i = self.hi % 4
        self.hi += 1
        return i

    def btile(self):
        i = self.bi % 4
        self.bi += 1
        return i

    def load_consts(self, vecs_d):
        S = self.S
        S.op("sync", lambda e: e.dma_start(out=self.vecs[:, :], in_=vecs_d), writes=["vecs"], dma=S.new_dma_sem())
        S.op("vector", lambda e: e.memset(self.ones[:, :], 1.0), writes=["ones"])
        S.op("vector", lambda e: e.memset(self.small[:, 0:1], EPS), writes=["small"])

    def norm_pass(self, src_fn, goff, want_ssq=True, halo_src=None, dst_fn=None, halo_dst=None,
                  router=None, out_dram_fn=None):
        S = self.S
        ps, ft, bt = self.ps, self.ft, self.bt
        if dst_fn is None:
            dst_fn = lambda c, tg: self.act3[:, c, tg * TG:(tg + 1) * TG]
        cols = [(tg, TG, tg * TG) for tg in range(NTG)]
        if halo_src is not None:
            cols.append(("h", 8, T))
        if want_ssq:
            for ci, (tg, wdt, roff) in enumerate(cols):
                bank = 6 + (ci % 2)
                for c in range(DC):
                    f = self.ftile()
                    src = src_fn(c, tg) if tg != "h" else halo_src(c)
                    S.op("sync", lambda e, f=f, src=src, wdt=wdt: e.dma_start(out=ft[:, f, 0:wdt], in_=src),
                         writes=[("ft", f)], dma="d_f%d" % f)
                    b = self.btile()
                    S.op("scalar", lambda e, f=f, b=b, wdt=wdt: e.activation(bt[:, b, 0:wdt], ft[:, f, 0:wdt], AF.Square),
                         reads=[("ft", f)], writes=[("bt", b)])
                    S.op("tensor", lambda e, b=b, c=c, bank=bank, wdt=wdt: e.matmul(
                        ps[:, bank, 0:wdt], self.ones[:, :], bt[:, b, 0:wdt], start=(c == 0), stop=(c == DC - 1)),
                        reads=[("bt", b), "ones"], writes=[("ps", bank)] if c in (0, DC - 1) else [], signal=True)
                f = self.ftile()
                S.op("scalar", lambda e, f=f, bank=bank, wdt=wdt: e.activation(
                    ft[:, f, 0:wdt], ps[:, bank, 0:wdt], AF.Sqrt, bias=self.small[:, 0:1], scale=1.0 / D),
                    reads=[("ps", bank), "small"], writes=[("ft", f)])
                S.op("vector", lambda e, f=f, roff=roff, wdt=wdt: e.reciprocal(self.rstd[:, roff:roff + wdt], ft[:, f, 0:wdt]),
                     reads=[("ft", f)], writes=[("rstd", tg)])
        for (tg, wdt, roff) in cols:
            for c in range(DC):
                f = self.ftile()
                src = src_fn(c, tg) if tg != "h" else halo_src(c)
                S.op("sync", lambda e, f=f, src=src, wdt=wdt: e.dma_start(out=ft[:, f, 0:wdt], in_=src),
                     writes=[("ft", f)], dma="d_f%d" % f)
                dst = dst_fn(c, tg) if tg != "h" else halo_dst(c)
                res = ("act", c, tg)
                if out_dram_fn is not None:
                    S.op("vector", lambda e, f=f, c=c, roff=roff, wdt=wdt: e.scalar_tensor_tensor(
                        out=ft[:, f, 0:wdt], in0=ft[:, f, 0:wdt], scalar=self.vecs[:, goff + c:goff + c + 1],
                        in1=self.rstd[:, roff:roff + wdt], op0=ALU.mult, op1=ALU.mult),
                        reads=[("rstd", tg), "vecs"], writes=[("ft", f)])
                    od = out_dram_fn(c, tg)
                    S.op("sync", lambda e, f=f, od=od: e.dma_start(out=od, in_=ft[:, f, :]),
                         reads=[("ft", f)], dma="d_f%d" % f)
                    continue
                if router is not None:
                    wr, lgT = router
                    S.op("vector", lambda e, f=f, c=c, roff=roff, wdt=wdt: e.scalar_tensor_tensor(
                        out=ft[:, f, 0:wdt], in0=ft[:, f, 0:wdt], scalar=self.vecs[:, goff + c:goff + c + 1],
                        in1=self.rstd[:, roff:roff + wdt], op0=ALU.mult, op1=ALU.mult),
                        reads=[("rstd", tg), "vecs"], writes=[("ft", f)])
                    S.op("scalar", lambda e, f=f, dst=dst: e.activation(dst, ft[:, f, :], AF.Copy),
                         reads=[("ft", f)], writes=[res])
                    S.op("tensor", lambda e, f=f, c=c: e.matmul(ps[0:8, 7, :], wr[:, c, :], ft[:, f, :],
                                                               start=(c == 0), stop=(c == DC - 1)),
                         reads=[("ft", f), "wr"], writes=[("ps", 7)] if c in (0, DC - 1) else [], signal=True)
                    if c == DC - 1:
                        S.op("scalar", lambda e, tg=tg: e.activation(lgT[0:8, tg * TG:(tg + 1) * TG], ps[0:8, 7, :], AF.Copy),
                             reads=[("ps", 7)], writes=["xlo"])
                    continue
                S.op("vector", lambda e, f=f, dst=dst, c=c, roff=roff, wdt=wdt: e.scalar_tensor_tensor(
                    out=dst, in0=ft[:, f, 0:wdt], scalar=self.vecs[:, goff + c:goff + c + 1],
                    in1=self.rstd[:, roff:roff + wdt], op0=ALU.mult, op1=ALU.mult),
                    reads=[("ft", f), ("rstd", tg), "vecs"], writes=[res])

    def load_w(self, slot, kcs, src, ncols=128):
        S = self.S
        dst = self.wsl[:, slot, 0:kcs * ncols].rearrange("p (c n) -> p c n", n=ncols)
        S.op("gpsimd", lambda e: e.dma_start(out=dst, in_=src.rearrange("(c p) n -> p c n", p=128)),
             writes=[("w", slot)], dma="d_w%d" % slot)

    def wview(self, slot, kcs, ncols=128):
        return self.wsl[:, slot, 0:kcs * ncols].rearrange("p (c n) -> p c n", n=ncols)

    def linear(self, groups, rhs_fn, epilogue, ntg=NTG, tg_list=None, pre_fn=None, act_reads=None):
        S = self.S
        ps = self.ps
        tg_list = list(range(ntg)) if tg_list is None else tg_list
        m = max(len(g) for g in groups)
        nsets = 2
        slot_ctr = [0]
        gslots = {}

        def issue_w(gi):
            sl = []
            for mem in groups[gi]:
                segs = []
                for (kcs, src, kind) in mem:
                    s = slot_ctr[0] % NSLOT
                    slot_ctr[0] += 1
                    self.load_w(s, kcs, src)
                    segs.append((s, kcs, kind))
                sl.append(segs)
            gslots[gi] = sl

        steps = [(gi, tg) for gi in range(len(groups)) for tg in tg_list]
        PF = 2
        for i in range(len(steps) + PF):
            if i < len(steps):
                gi, tg = steps[i]
                if pre_fn is not None:
                    pre_fn(gi, tg)
            j = i - PF
            if j < 0:
                continue
            gi, tg = steps[j]
            if tg == tg_list[0]:
                issue_w(gi)
            setb = (j % nsets) * 3
            banks = []
            for mi, segs in enumerate(gslots[gi]):
                bank = setb + mi
                banks.append(bank)
                nmm = sum(k for (_, k, _) in segs)
                q = 0
                for (s, kcs, kind) in segs:
                    wv = self.wview(s, kcs)
                    for kc in range(kcs):
                        rhs, rres = rhs_fn(kind, q if kind == "act" else kc, tg)
                        S.op("tensor", lambda e, wv=wv, kc=kc, rhs=rhs, bank=bank, q=q, nmm=nmm: e.matmul(
                            ps[:, bank, :], wv[:, kc, :], rhs, start=(q == 0), stop=(q == nmm - 1)),
                            reads=[("w", s)] + rres, writes=[("ps", bank)] if q in (0, nmm - 1) else [],
                            signal=(q == nmm - 1))
                        q += 1
            epilogue(gi, tg, banks)

    def store_bf(self, b, dst, res):
        S = self.S
        return S.op("sync", lambda e: e.dma_start(out=dst, in_=self.bt[:, b, :]),
                    reads=[("bt", b)], writes=[res], dma="d_b%d" % b)

    def make_residual(self, src_fn, dst_fn, name):
        S = self.S
        ht, ps, ft = self.ht, self.ps, self.ft
        tiles = {}

        def pre(gi, tg):
            f = self.htile()
            tiles[(gi, tg)] = f
            src = src_fn(gi, tg)
            S.op("sync", lambda e: e.dma_start(out=ht[:, f, :], in_=src),
                 reads=[(name, gi, tg)], writes=[("ht", f)], dma="d_h%d" % f)

        def epi(gi, tg, banks, add_ft=None):
            f = tiles.pop((gi, tg))
            if add_ft is None:
                S.op("vector", lambda e: e.tensor_tensor(out=ht[:, f, :], in0=ht[:, f, :], in1=ps[:, banks[0], :], op=ALU.add),
                     reads=[("ps", banks[0])], writes=[("ht", f)])
            else:
                S.op("vector", lambda e: e.tensor_tensor(out=ht[:, f, :], in0=ht[:, f, :], in1=ft[:, add_ft, :], op=ALU.add),
                     reads=[("ft", add_ft)], writes=[("ht", f)])
            dst = dst_fn(gi, tg)
            S.op("sync", lambda e: e.dma_start(out=dst, in_=ht[:, f, :]),
                 reads=[("ht", f)], writes=[(name, gi, tg)], dma="d_h%d" % f)
        return pre, epi


def _tile(ap2d, c, tg):
    return ap2d[c * 128:(c + 1) * 128, tg * TG:(tg + 1) * TG]


class _Stop(Exception):
    pass


def build_A(upto=99, dbg=False):
    nc = bass.Bass("TRN2", target_bir_lowering=False)
    dt = lambda n, s, d, k: nc.dram_tensor(n, s, d, kind=k).ap()
    xT = dt("xT", [D, T], F32, "ExternalInput")
    xh = dt("xh", [D, 8], F32, "ExternalInput")
    pT = dt("pT", [PLE, T], F32, "ExternalInput")
    vecs_d = dt("vecs", [128, NV], F32, "ExternalInput")
    w_in = dt("w_in", [D, 3 * D], F32, "ExternalInput")
    w_out = dt("w_out", [D, D], F32, "ExternalInput")
    w_gu = dt("w_gu", [D, 2 * DFF], F32, "ExternalInput")
    w_dn = dt("w_dn", [DFF, D], F32, "ExternalInput")
    w_pg = dt("w_pg", [D, D], F32, "ExternalInput")
    w_pu = dt("w_pu", [PLE, D], F32, "ExternalInput")
    w_kv = dt("w_kv", [D, 2 * D], F32, "ExternalInput")
    w_q = dt("w_q", [D, D], F32, "ExternalInput")
    HT = dt("HT", [D, T], F32, "ExternalOutput")
    QT = dt("QT", [D, T], BF16, "ExternalOutput")
    KT = dt("KT", [D, T], BF16, "ExternalOutput")
    VV = dt("VV", [T, D], BF16, "ExternalOutput")
    GT = dt("GT", [D, T], BF16, "Internal")
    FA = dt("FA", [DFF, T], BF16, "Internal")
    with ExitStack() as st:
        B = Builder(nc, st)
        S = B.S
        ps, ft, bt, act3 = B.ps, B.ft, B.bt, B.act3
        B.load_consts(vecs_d)
        DBG = dt("DBG", [128, DC * T], BF16, "ExternalOutput") if dbg else None

        def chk(k):
            if upto == k:
                if dbg:
                    S.barrier()
                    for q in range(4):
                        S.op("sync", lambda e, q=q: e.dma_start(out=DBG[:, q * 16384:(q + 1) * 16384], in_=B.act[:, q * 16384:(q + 1) * 16384]),
                             dma="d_a%d" % q)
                raise _Stop()
        try:
            build_A_body(B, chk, locals())
        except _Stop:
            pass
        finish(B)
        with nc.Block() as block:
            S.emit(block)
    return nc


def build_A_body(B, chk, L):
    if True:
        S = B.S
        ps, ft, bt, act3 = B.ps, B.ft, B.bt, B.act3
        xT, xh, pT, w_in, w_out, w_gu, w_dn, w_pg, w_pu, w_kv, w_q = [L[k] for k in
            ("xT", "xh", "pT", "w_in", "w_out", "w_gu", "w_dn", "w_pg", "w_pu", "w_kv", "w_q")]
        HT, QT, KT, VV, GT, FA = [L[k] for k in ("HT", "QT", "KT", "VV", "GT", "FA")]
        acth = B.xar_bf[:, 0:DC * 8].rearrange("p (c t) -> p c t", t=8)
        pbuf = B.xar_bf[:, 4096:4096 + 2 * T].rearrange("p (c t) -> p c t", t=T)

        def act_rhs(kind, kc, tg):
            if kind == "act":
                return act3[:, kc, tg * TG:(tg + 1) * TG], [("act", kc, tg)]
            if kind == "p":
                return pbuf[:, kc, tg * TG:(tg + 1) * TG], ["pbuf"]
            raise ValueError(kind)

        B.norm_pass(lambda c, tg: _tile(xT, c, tg), V_ANORM,
                    halo_src=lambda c: xh[c * 128:(c + 1) * 128, :],
                    halo_dst=lambda c: acth[:, c, :])
        chk(0)
        groups = []
        for n in range(DC):
            groups.append([[(DC, w_in[:, m * D + n * 128: m * D + (n + 1) * 128], "act")] for m in (1, 2, 0)])
        uc = B.xar[:, 384:384 + 2 * 520].rearrange("p (a t) -> p a t", t=520)
        uch_all = B.xar[:, 128:128 + DC * 8].rearrange("p (c t) -> p c t", t=8)

        def epi_conv(gi, tg, banks):
            bc, bu, bb = banks
            uch = uch_all[:, gi, :]
            f = B.ftile()
            S.op("scalar", lambda e: e.activation(ft[:, f, :], ps[:, bc, :], AF.Copy),
                 reads=[("ps", bc)], writes=[("ft", f)])
            u = (gi * NTG + tg) % 2
            S.op("vector", lambda e: e.tensor_tensor(out=uc[:, u, 2:2 + TG], in0=ps[:, bu, :], in1=ft[:, f, :], op=ALU.mult),
                 reads=[("ps", bu), ("ft", f)], writes=[("uc", u)])
            S.op("vector", lambda e: e.tensor_copy(uc[:, u, 0:2], uch[:, 2 * tg:2 * tg + 2]),
                 reads=[("uch", gi)], writes=[("uc", u)])
            f2 = B.ftile()
            S.op("vector", lambda e: e.tensor_scalar(ft[:, f2, :], uc[:, u, 0:TG], B.vecs[:, V_CW0 + gi:V_CW0 + gi + 1], None, op0=ALU.mult),
                 reads=[("uc", u), "vecs"], writes=[("ft", f2)])
            S.op("vector", lambda e: e.scalar_tensor_tensor(out=ft[:, f2, :], in0=uc[:, u, 1:1 + TG], scalar=B.vecs[:, V_CW1 + gi:V_CW1 + gi + 1],
                 in1=ft[:, f2, :], op0=ALU.mult, op1=ALU.add), reads=[("uc", u)], writes=[("ft", f2)])
            S.op("vector", lambda e: e.scalar_tensor_tensor(out=ft[:, f2, :], in0=uc[:, u, 2:2 + TG], scalar=B.vecs[:, V_CW2 + gi:V_CW2 + gi + 1],
                 in1=ft[:, f2, :], op0=ALU.mult, op1=ALU.add), reads=[("uc", u)], writes=[("ft", f2)])
            b = B.btile()
            S.op("vector", lambda e: e.tensor_tensor(out=bt[:, b, :], in0=ps[:, bb, :], in1=ft[:, f2, :], op=ALU.mult),
                 reads=[("ps", bb), ("ft", f2)], writes=[("bt", b)])
            B.store_bf(b, _tile(GT, gi, tg), ("GT", gi, tg))

        B_halo_bank = 6
        slot_i = [0]
        for n in range(DC):
            for mi, m in enumerate((1, 2)):
                s = slot_i[0] % NSLOT
                slot_i[0] += 1
                B.load_w(s, DC, w_in[:, m * D + n * 128: m * D + (n + 1) * 128])
                wv = B.wview(s, DC)
                for kc in range(DC):
                    S.op("tensor", lambda e, wv=wv, kc=kc, mi=mi: e.matmul(
                        ps[:, B_halo_bank, mi * 8:mi * 8 + 8], wv[:, kc, :], acth[:, kc, :], start=(kc == 0), stop=(kc == DC - 1)),
                        reads=[("w", s), ("act", kc, "h")], writes=[("ps", B_halo_bank)] if kc in (0, DC - 1) else [],
                        signal=(kc == DC - 1))
            S.op("scalar", lambda e: e.activation(B.small[:, 8:16], ps[:, B_halo_bank, 0:8], AF.Copy),
                 reads=[("ps", B_halo_bank)], writes=["hc"])
            S.op("vector", lambda e, n=n: e.tensor_tensor(out=uch_all[:, n, :], in0=ps[:, B_halo_bank, 8:16], in1=B.small[:, 8:16], op=ALU.mult),
                 reads=[("ps", B_halo_bank), "hc"], writes=[("uch", n)])

        B.linear(groups, act_rhs, epi_conv)

        chk(1)
        S.barrier()
        load_act(B, GT, DC)
        pre, epi = B.make_residual(lambda gi, tg: _tile(xT, gi, tg), lambda gi, tg: _tile(HT, gi, tg), "H")
        B.linear([[[(DC, w_out[:, n * 128:(n + 1) * 128], "act")]] for n in range(DC)], act_rhs, epi, pre_fn=pre)

        chk(2)
        S.barrier()
        B.norm_pass(lambda c, tg: _tile(HT, c, tg), V_FFN0)

        def epi_swiglu(dst, name):
            def epi(gi, tg, banks):
                bg, bu = banks
                f = B.ftile()
                S.op("scalar", lambda e: e.activation(ft[:, f, :], ps[:, bg, :], AF.Silu),
                     reads=[("ps", bg)], writes=[("ft", f)])
                b = B.btile()
                S.op("vector", lambda e: e.tensor_tensor(out=bt[:, b, :], in0=ps[:, bu, :], in1=ft[:, f, :], op=ALU.mult),
                     reads=[("ps", bu), ("ft", f)], writes=[("bt", b)])
                B.store_bf(b, _tile(dst, gi, tg), (name, gi, tg))
            return epi

        B.linear([[[(DC, w_gu[:, n * 128:(n + 1) * 128], "act")], [(DC, w_gu[:, DFF + n * 128:DFF + (n + 1) * 128], "act")]]
                  for n in range(FC)], act_rhs, epi_swiglu(FA, "FA"))

        chk(4)
        S.barrier()
        actf = B.act[:, 0:FC * TG].rearrange("p (c t) -> p c t", t=TG)
        segs = [(0, 32), (32, 32), (64, 22)]
        for tg in range(NTG):
            for q in range(4):
                c0, c1 = q * 22, min(FC, (q + 1) * 22)
                S.op("sync", lambda e, c0=c0, c1=c1, tg=tg: e.dma_start(
                    out=actf[:, c0:c1, :], in_=FA[c0 * 128:c1 * 128, tg * TG:(tg + 1) * TG].rearrange("(c p) t -> p c t", p=128)),
                    writes=[("actf", q)], dma="d_a%d" % q)

            def rhs_f(kind, kc, tgx):
                return actf[:, kc, :], [("actf", kc // 22)]
            pre, epi = B.make_residual(lambda gi, tgx: _tile(HT, gi, tgx), lambda gi, tgx: _tile(HT, gi, tgx), "H")
            B.linear([[[(kn, w_dn[k0 * 128:(k0 + kn) * 128, n * 128:(n + 1) * 128], "act") for (k0, kn) in segs]]
                      for n in range(DC)], rhs_f, epi, tg_list=[tg], pre_fn=pre)

        chk(5)
        S.barrier()
        B.norm_pass(lambda c, tg: _tile(HT, c, tg), V_PLE0)
        ple_stage(B, act_rhs, pbuf, pT, w_pg, w_pu, HT)

        chk(7)
        S.barrier()
        B.norm_pass(lambda c, tg: _tile(HT, c, tg), V_KV)

        def epi_copy(dst, name, scale=None):
            def epi(gi, tg, banks):
                b = B.btile()
                if scale is None:
                    S.op("scalar", lambda e: e.activation(bt[:, b, :], ps[:, banks[0], :], AF.Copy),
                         reads=[("ps", banks[0])], writes=[("bt", b)])
                else:
                    S.op("scalar", lambda e: e.activation(bt[:, b, :], ps[:, banks[0], :], AF.Copy, scale=scale),
                         reads=[("ps", banks[0])], writes=[("bt", b)])
                B.store_bf(b, _tile(dst, gi, tg), (name, gi, tg))
            return epi

        B.linear([[[(DC, w_kv[:, n * 128:(n + 1) * 128], "act")]] for n in range(DC)], act_rhs, epi_copy(KT, "KT"))
        sl = [0]
        for ng in range(8):
            slots = []
            for ks in range(4):
                s = sl[0] % NSLOT
                sl[0] += 1
                B.load_w(s, 8, w_kv[ks * 1024:(ks + 1) * 1024, D + ng * 512:D + (ng + 1) * 512], ncols=512)
                slots.append(s)
            for tb in range(16):
                bank = (ng * 16 + tb) % 6
                for kc in range(DC):
                    s = slots[kc // 8]
                    wv = B.wview(s, 8, ncols=512)
                    S.op("tensor", lambda e, wv=wv, kc=kc, tb=tb, bank=bank: e.matmul(
                        ps[:, bank, :], act3[:, kc, tb * 128:(tb + 1) * 128], wv[:, kc % 8, :], start=(kc == 0), stop=(kc == DC - 1)),
                        reads=[("w", s), ("act", kc, tb // 4)], writes=[("ps", bank)] if kc in (0, DC - 1) else [], signal=(kc == DC - 1))
                b = B.btile()
                S.op("scalar" if tb % 2 else "vector",
                     (lambda e, b=b, bank=bank: e.activation(bt[:, b, :], ps[:, bank, :], AF.Copy)) if tb % 2 else
                     (lambda e, b=b, bank=bank: e.tensor_copy(bt[:, b, :], ps[:, bank, :])),
                     reads=[("ps", bank)], writes=[("bt", b)])
                B.store_bf(b, VV[tb * 128:(tb + 1) * 128, ng * 512:(ng + 1) * 512], ("VV", ng, tb))
        B.norm_pass(lambda c, tg: _tile(HT, c, tg), V_BN, want_ssq=False)
        B.linear([[[(DC, w_q[:, n * 128:(n + 1) * 128], "act")]] for n in range(DC)], act_rhs,
                 epi_copy(QT, "QT", scale=128.0 ** -0.5))


def finish(B):
    S = B.S
    S.wait_all("sync", list(S.dma_last.values()))


def load_act(B, src, kc_total):
    S = B.S
    per = kc_total // 4
    for q in range(4):
        S.op("sync", lambda e, q=q: e.dma_start(
            out=B.act3[:, q * per:(q + 1) * per, :],
            in_=src[q * per * 128:(q + 1) * per * 128, :].rearrange("(c p) t -> p c t", p=128)),
            writes=[("act", c, tg) for c in range(q * per, (q + 1) * per) for tg in list(range(NTG))],
            dma="d_a%d" % q)


def ple_stage(B, act_rhs, pbuf, pT, w_pg, w_pu, HT):
    S = B.S
    ps, ft = B.ps, B.ft
    S.op("gpsimd", lambda e: e.dma_start(out=pbuf, in_=pT.rearrange("(c p) t -> p c t", p=128)),
         writes=["pbuf"], dma=S.new_dma_sem())
    pre, epi_res = B.make_residual(lambda gi, tg: _tile(HT, gi, tg), lambda gi, tg: _tile(HT, gi, tg), "H")

    def epi(gi, tg, banks):
        bg, bu = banks
        f = B.ftile()
        S.op("scalar", lambda e: e.activation(ft[:, f, :], ps[:, bg, :], AF.Sigmoid),
             reads=[("ps", bg)], writes=[("ft", f)])
        S.op("vector", lambda e: e.tensor_tensor(out=ft[:, f, :], in0=ps[:, bu, :], in1=ft[:, f, :], op=ALU.mult),
             reads=[("ps", bu)], writes=[("ft", f)])
        epi_res(gi, tg, banks, add_ft=f)

    B.linear([[[(DC, w_pg[:, n * 128:(n + 1) * 128], "act")], [(2, w_pu[:, n * 128:(n + 1) * 128], "p")]]
              for n in range(DC)], act_rhs, epi, pre_fn=pre)


def _vec_cols(v):
    return np.ascontiguousarray(np.asarray(v, np.float32).reshape(DC, 128).T)


def _tok_index(c):
    return np.concatenate([np.arange(512 * (4 * j + c), 512 * (4 * j + c) + 512) for j in range(4)])


def prep_A(r, inp):
    b, c = r // 4, r % 4
    toks = _tok_index(c)
    x = inp["x"]
    xT = np.ascontiguousarray(x[b, toks, :].T)
    xh = np.zeros((D, 8), np.float32)
    for j in range(4):
        s0 = 512 * (4 * j + c)
        if s0 >= 2:
            xh[:, 2 * j:2 * j + 2] = x[b, s0 - 2:s0, :].T
    vec_list = [inp["a_norm"][0], inp["a_conv_w"][0, 0], inp["a_conv_w"][0, 1], inp["a_conv_w"][0, 2],
                inp["ffn_norm"][0], inp["ple_norm"][0], inp["kv_norm"], inp["b_norm"][0],
                inp["ffn_norm"][1], inp["ple_norm"][1], inp["final_norm"]]
    vecs = np.ascontiguousarray(np.concatenate([_vec_cols(v) for v in vec_list], axis=1))
    return {
        "xT": xT, "xh": xh, "pT": np.ascontiguousarray(inp["p"][0, b, toks, :].T), "vecs": vecs,
        "w_in": inp["a_w_in"][0], "w_out": inp["a_w_out"][0], "w_gu": inp["dense_w_gu"][0],
        "w_dn": inp["dense_w_down"][0], "w_pg": inp["ple_w_gate"][0], "w_pu": inp["ple_w_up"][0],
        "w_kv": inp["w_kv"], "w_q": inp["b_w_q"][0],
    }


def build_B(upto=99):
    nc = bass.Bass("TRN2", target_bir_lowering=False)
    dt = lambda n, s, d, k: nc.dram_tensor(n, s, d, kind=k).ap()
    HT = dt("HT", [D, T], F32, "ExternalInput")
    QT = dt("QT", [D, T], BF16, "ExternalInput")
    KTall = dt("KTall", [4, D, T], BF16, "ExternalInput")
    VVall = dt("VVall", [4, T, D], BF16, "ExternalInput")
    masks_d = dt("masks", [128, 16 * TG], F32, "ExternalInput")
    tri_d = dt("tri", [128, 256], F32, "ExternalInput")
    ident_d = dt("ident", [128, 128], F32, "ExternalInput")
    pT = dt("pT", [PLE, T], F32, "ExternalInput")
    vecs_d = dt("vecs", [128, NV], F32, "ExternalInput")
    w_o = dt("w_o", [D, D], F32, "ExternalInput")
    wr_d = dt("wr", [128, DC * NE], F32, "ExternalInput")
    br_d = dt("br", [128, 128], F32, "ExternalInput")
    w_egu = dt("w_egu", [NE, D, 2 * D], F32, "ExternalInput")
    w_edn = dt("w_edn", [NE, D, D], F32, "ExternalInput")
    w_pg = dt("w_pg", [D, D], F32, "ExternalInput")
    w_pu = dt("w_pu", [PLE, D], F32, "ExternalInput")
    YT = dt("YT", [D, T], F32, "ExternalOutput")
    H2 = dt("H2", [D, T], F32, "Internal")
    AT = dt("AT", [D, T], BF16, "Internal")
    HE = dt("HE", [NE, D, T], BF16, "Internal")
    with ExitStack() as st:
        B = Builder(nc, st)
        S = B.S
        B.load_consts(vecs_d)
        try:
            build_B_body(B, locals(), upto)
        except _Stop:
            pass
        finish(B)
        with nc.Block() as block:
            S.emit(block)
    return nc


def build_B_body(B, L, upto):
    S = B.S
    nc = B.nc
    ps, ft, bt, act3, act = B.ps, B.ft, B.bt, B.act3, B.act
    HT, QT, KTall, VVall, masks_d, tri_d, ident_d, pT, w_o, wr_d, br_d, w_egu, w_edn, w_pg, w_pu, YT, H2, AT, HE = [
        L[k] for k in ("HT", "QT", "KTall", "VVall", "masks_d", "tri_d", "ident_d", "pT", "w_o", "wr_d", "br_d",
                       "w_egu", "w_edn", "w_pg", "w_pu", "YT", "H2", "AT", "HE")]
    small = B.small
    ident = B.st.enter_context(nc.sbuf_tensor("s_ident", [128, 128], F32))
    wr = B.st.enter_context(nc.sbuf_tensor("s_wr", [128, DC, NE], F32))
    S.op("sync", lambda e: e.dma_start(out=ident[:, :], in_=ident_d), writes=["ident"], dma=S.new_dma_sem())
    S.op("sync", lambda e: e.dma_start(out=wr[:, :, :], in_=wr_d.rearrange("p (c n) -> p c n", n=NE)), writes=["wr"], dma=S.new_dma_sem())
    S.op("vector", lambda e: e.memset(small[:, 1:2], 1.0), writes=["one"])
    pbuf = B.xar_bf[:, 4096:4096 + 2 * T].rearrange("p (c t) -> p c t", t=T)

    def act_rhs(kind, kc, tg):
        if kind == "act":
            return act3[:, kc, tg * TG:(tg + 1) * TG], [("act", kc, tg)]
        return pbuf[:, kc, tg * TG:(tg + 1) * TG], ["pbuf"]

    actf32 = act.bitcast(F32)
    kbuf = [act[:, i * 8192:(i + 1) * 8192] for i in range(2)]
    vbuf = [act[:, 16384 + i * 8192:16384 + (i + 1) * 8192].rearrange("p (n d) -> p n d", d=128) for i in range(2)]
    qbuf = [act[:, 32768 + i * 2048:32768 + (i + 1) * 2048] for i in range(2)]
    mask = act[:, 36864:45056].rearrange("p (m t) -> p m t", t=TG)
    negtri = act[:, 45056:45184]
    negones = act[:, 45184:45312]
    spr = [act[:, 45312 + i * 512:45312 + (i + 1) * 512] for i in range(3)]
    wtr = [act[:, 46848 + i * 512:46848 + (i + 1) * 512] for i in range(3)]
    ssb = [act[:, 48384 + i * 512:48384 + (i + 1) * 512] for i in range(3)]
    e1r = [actf32[:, 24960 + i * 512:24960 + (i + 1) * 512] for i in range(2)]
    ssum = actf32[:, 25984:26496]
    S.op("gpsimd", lambda e: e.dma_start(out=mask, in_=masks_d.rearrange("p (m t) -> p m t", t=TG)), writes=["mask"], dma=S.new_dma_sem())
    S.op("gpsimd", lambda e: e.dma_start(out=act[:, 45056:45312], in_=tri_d), writes=["tri"], dma=S.new_dma_sem())

    def load_head(h):
        hb = h % 2
        kv4 = kbuf[hb].rearrange("p (j c t) -> p j c t", c=4, t=TG)
        vv5 = vbuf[hb].rearrange("p (j c i) d -> p j c i d", c=4, i=4)
        for c in range(4):
            S.op("sync", lambda e, c=c: e.dma_start(
                out=kv4[:, :, c, :], in_=KTall[c, h * 128:(h + 1) * 128, :].rearrange("p (j t) -> p j t", t=TG)),
                writes=[("kbuf", hb)], dma="d_k%d_%d" % (hb, c))
            for j in range(4):
                S.op("sync", lambda e, c=c, j=j: e.dma_start(
                    out=vv5[:, j, c, :, :],
                    in_=VVall[c, j * TG:(j + 1) * TG, h * 128:(h + 1) * 128].rearrange("(i p) d -> p i d", p=128)),
                    writes=[("vbuf", hb)], dma="d_v%d_%d" % (hb, c * 4 + j))
        S.op("sync", lambda e: e.dma_start(out=qbuf[hb], in_=QT[h * 128:(h + 1) * 128, :]),
             writes=[("qbuf", hb)], dma="d_q%d" % hb)

    NH = 32 if upto != 50 else 1
    steps = []
    for h in range(NH):
        for j in range(4):
            nb = 16 * (j + 1)
            for n in range(nb):
                steps.append((h, j, nb - 1 - n, n, nb))
    import os as _os
    if _os.environ.get('ATT_STEPS'):
        steps = steps[:int(_os.environ['ATT_STEPS'])]
    NS = len(steps)

    def stage1(i):
        h, j, kb, n, nb = steps[i]
        hb = h % 2
        zb = i % 2
        S.op("tensor", lambda e: e.matmul(ps[:, zb, :], kbuf[hb][:, kb * 128:(kb + 1) * 128], qbuf[hb][:, j * TG:(j + 1) * TG],
                                          start=True, stop=True),
             reads=[("kbuf", hb), ("qbuf", hb)], writes=[("ps", zb)])
        e1 = e1r[i % 2]
        S.op("scalar", lambda e: e.activation(e1, ps[:, zb, :], AF.Exp), reads=[("ps", zb)], writes=[("e1", i % 2)])
        sp = spr[i % 3]
        masked = kb >= 16 * j
        if masked:
            mi = kb - 16 * j
            S.op("scalar", lambda e: e.activation(e1, e1, AF.Ln, bias=small[:, 1:2]), reads=["one"], writes=[("e1", i % 2)])
            S.op("vector", lambda e: e.tensor_tensor(out=sp, in0=e1, in1=mask[:, mi, :], op=ALU.mult),
                 reads=[("e1", i % 2), "mask"], writes=[("sp", i % 3)])
        else:
            S.op("scalar", lambda e: e.activation(sp, e1, AF.Ln, bias=small[:, 1:2]), reads=["one", ("e1", i % 2)], writes=[("sp", i % 3)])
        if n < nb - 1:
            if n == 0:
                S.op("vector", lambda e: e.tensor_copy(ssum, sp), reads=[("sp", i % 3)], writes=["ssum"])
            else:
                S.op("vector", lambda e: e.tensor_tensor(out=ssum, in0=ssum, in1=sp, op=ALU.add), reads=[("sp", i % 3)], writes=["ssum"])
            S.op("vector", lambda e: e.tensor_copy(ssb[(i + 1) % 3], ssum), reads=["ssum"], writes=[("ssb", (i + 1) % 3)])

    def stage2(i):
        h, j, kb, n, nb = steps[i]
        hb = h % 2
        eb = 2 + i % 2
        sp = spr[i % 3]
        last = "tri" if n == 0 else "ones"
        S.op("tensor", lambda e: e.matmul(ps[:, eb, :], kbuf[hb][:, kb * 128:(kb + 1) * 128], qbuf[hb][:, j * TG:(j + 1) * TG],
                                          start=True, stop=False),
             reads=[("kbuf", hb), ("qbuf", hb)], writes=[("ps", eb)], signal=False)
        S.op("tensor", lambda e: e.matmul(ps[:, eb, :], negtri, sp, start=False, stop=(n == 0)),
             reads=[("sp", i % 3), "tri"], writes=[("ps", eb)] if n == 0 else [], signal=(n == 0))
        if n > 0:
            S.op("tensor", lambda e: e.matmul(ps[:, eb, :], negones, ssb[i % 3], start=False, stop=True),
                 reads=[("ssb", i % 3), "tri"], writes=[("ps", eb)])
        wt = wtr[i % 3]
        S.op("scalar", lambda e: e.activation(wt, ps[:, eb, :], AF.Exp), reads=[("ps", eb)], writes=[("wt", i % 3)])
        if kb >= 16 * j:
            mi = kb - 16 * j
            S.op("vector", lambda e: e.tensor_tensor(out=wt, in0=wt, in1=mask[:, mi, :], op=ALU.mult),
                 reads=["mask"], writes=[("wt", i % 3)])

    def stage3(i):
        h, j, kb, n, nb = steps[i]
        hb = h % 2
        ob = 4 + (h * 4 + j) % 2
        wt = wtr[i % 3]
        S.op("tensor", lambda e: e.matmul(ps[:, ob, :], vbuf[hb][:, kb, :], wt, start=(n == 0), stop=(n == nb - 1)),
             reads=[("vbuf", hb), ("wt", i % 3)], writes=[("ps", ob)] if n in (0, nb - 1) else [], signal=(n == nb - 1))
        if n == nb - 1:
            b = B.btile()
            S.op("vector", lambda e: e.tensor_copy(bt[:, b, :], ps[:, ob, :]), reads=[("ps", ob)], writes=[("bt", b)])
            B.store_bf(b, _tile(AT, h, j), ("AT", h, j))

    if upto >= 50:
        load_head(0)
        for i in range(NS + 2):
            if i < NS:
                h, j, kb, n, nb = steps[i]
                if j == 0 and n == 0 and h + 1 < NH:
                    load_head(h + 1)
                stage1(i)
            if 0 <= i - 1 < NS:
                stage2(i - 1)
            if 0 <= i - 2 < NS:
                stage3(i - 2)
    if upto == 50:
        raise _Stop()

    S.barrier()
    load_act(B, AT, DC)
    pre, epi = B.make_residual(lambda gi, tg: _tile(HT, gi, tg), lambda gi, tg: _tile(H2, gi, tg), "H")
    B.linear([[[(DC, w_o[:, n * 128:(n + 1) * 128], "act")]] for n in range(DC)], act_rhs, epi, pre_fn=pre)
    if upto == 51:
        raise _Stop()

    S.barrier()
    xar = B.xar
    lgT = xar[:, 0:T]
    gb = xar[:, 0:T]
    lg = xar[:, 2048:2176]
    mx = xar[:, 2176:2304]
    ee = xar[:, 2304:2432]
    sel = xar[:, 2432:2560]
    gates = xar[:, 2560:2688]
    den = xar[:, 2688:2704]
    rden = xar[:, 2704:2720]
    brep = xar[:, 2720:2848]
    S.op("sync", lambda e: e.dma_start(out=brep, in_=br_d), writes=["brep"], dma=S.new_dma_sem())
    B.norm_pass(lambda c, tg: _tile(H2, c, tg), V_FFN1, router=(wr, lgT))
    for tb in range(16):
        S.op("tensor", lambda e, tb=tb: e.matmul(ps[:, 6, tb * 8:(tb + 1) * 8], lgT[0:8, tb * 128:(tb + 1) * 128], ident[0:8, 0:8],
                                                 start=True, stop=True),
             reads=["xlo", "ident"], writes=[("ps", 6)])
    S.op("vector", lambda e: e.tensor_tensor(out=lg, in0=ps[:, 6, 0:128], in1=brep, op=ALU.add),
         reads=[("ps", 6), "brep"], writes=["lg"])
    for tb in range(16):
        sl = slice(tb * 8, tb * 8 + 8)
        S.op("vector", lambda e, sl=sl: e.max(out=mx[:, sl], in_=lg[:, sl]), reads=["lg"], writes=["mx"])
    for tb in range(16):
        sl = slice(tb * 8, tb * 8 + 8)
        S.op("vector", lambda e, sl=sl, tb=tb: e.tensor_scalar(ee[:, sl], lg[:, sl], mx[:, tb * 8:tb * 8 + 1], None, op0=ALU.subtract),
             reads=["lg", "mx"], writes=["ee"])
        S.op("vector", lambda e, sl=sl, tb=tb: e.tensor_scalar(sel[:, sl], lg[:, sl], mx[:, tb * 8 + 1:tb * 8 + 2], None, op0=ALU.is_ge),
             reads=["lg", "mx"], writes=["sel"])
    S.op("scalar", lambda e: e.activation(ee, ee, AF.Exp), reads=[], writes=["ee"])
    S.op("vector", lambda e: e.tensor_tensor(out=ee, in0=ee, in1=sel, op=ALU.mult), reads=["sel"], writes=["ee"])
    S.op("vector", lambda e: e.tensor_reduce(out=den[:, 0:16], in_=ee.rearrange("p (a b) -> p a b", b=8), axis=AX.X, op=ALU.add),
         reads=["ee"], writes=["den"])
    S.op("vector", lambda e: e.reciprocal(rden[:, 0:16], den[:, 0:16]), reads=["den"], writes=["rden"])
    for tb in range(16):
        sl = slice(tb * 8, tb * 8 + 8)
        S.op("vector", lambda e, sl=sl, tb=tb: e.tensor_scalar(gates[:, sl], ee[:, sl], rden[:, tb:tb + 1], None, op0=ALU.mult),
             reads=["ee", "rden"], writes=["gates"])
    if upto == 52:
        raise _Stop()

    for ex in range(NE):
        for tg in range(NTG):
            for tl in range(4):
                tb = tg * 4 + tl
                f = B.ftile()
                S.op("vector", lambda e, f=f, tb=tb, ex=ex: e.tensor_copy(
                    ft[:, f, 0:128], gates[:, tb * 8 + ex:tb * 8 + ex + 1].to_broadcast([128, 128])),
                    reads=["gates"], writes=[("ft", f)])
                S.op("tensor", lambda e, f=f, tl=tl: e.matmul(ps[:, 6, tl * 128:(tl + 1) * 128], ft[:, f, 0:128], ident[:, :],
                                                             start=True, stop=True),
                     reads=[("ft", f), "ident"], writes=[("ps", 6)])
            S.op("scalar", lambda e, tg=tg: e.activation(gb[:, tg * TG:(tg + 1) * TG], ps[:, 6, :], AF.Copy),
                 reads=[("ps", 6)], writes=["xlo"])

        def epi_e(gi, tg, banks, ex=ex):
            bg, bu = banks
            f = B.ftile()
            S.op("scalar", lambda e: e.activation(ft[:, f, :], ps[:, bg, :], AF.Silu), reads=[("ps", bg)], writes=[("ft", f)])
            S.op("vector", lambda e: e.tensor_tensor(out=ft[:, f, :], in0=ps[:, bu, :], in1=ft[:, f, :], op=ALU.mult),
                 reads=[("ps", bu)], writes=[("ft", f)])
            b = B.btile()
            S.op("vector", lambda e: e.tensor_tensor(out=bt[:, b, :], in0=ft[:, f, :], in1=gb[:, tg * TG:(tg + 1) * TG], op=ALU.mult),
                 reads=[("ft", f), "xlo"], writes=[("bt", b)])
            B.store_bf(b, _tile(HE[ex], gi, tg), ("HE", ex, gi, tg))

        B.linear([[[(DC, w_egu[ex][:, n * 128:(n + 1) * 128], "act")], [(DC, w_egu[ex][:, D + n * 128:D + (n + 1) * 128], "act")]]
                  for n in range(DC)], act_rhs, epi_e)
    if upto == 53:
        raise _Stop()
    for ex in range(NE):
        S.barrier()
        load_act(B, HE[ex], DC)
        pre, epi = B.make_residual(lambda gi, tg: _tile(H2, gi, tg), lambda gi, tg: _tile(H2, gi, tg), "H")
        B.linear([[[(DC, w_edn[ex][:, n * 128:(n + 1) * 128], "act")]] for n in range(DC)], act_rhs, epi, pre_fn=pre)
    if upto == 54:
        raise _Stop()
    S.barrier()
    B.norm_pass(lambda c, tg: _tile(H2, c, tg), V_PLE1)
    ple_stage(B, act_rhs, pbuf, pT, w_pg, w_pu, H2)
    S.barrier()
    B.norm_pass(lambda c, tg: _tile(H2, c, tg), V_FIN, out_dram_fn=lambda c, tg: _tile(YT, c, tg))


def make_masks(c):
    m = np.zeros((16, 128, TG), np.float32)
    k = np.arange(128)[:, None]
    q = np.arange(TG)[None, :]
    for o in range(4):
        for i in range(4):
            if o < c:
                m[o * 4 + i] = 1.0
            elif o == c:
                m[o * 4 + i] = ((i * 128 + k) < q).astype(np.float32)
    return np.ascontiguousarray(m.transpose(1, 0, 2).reshape(128, 16 * TG))


def consts_B():
    kp = np.arange(128)[:, None]
    k = np.arange(128)[None, :]
    negtri = -(kp >= k).astype(np.float32)
    tri = np.concatenate([negtri, -np.ones((128, 128), np.float32)], axis=1)
    return np.ascontiguousarray(tri), np.eye(128, dtype=np.float32)


def prep_B(r, inp, outA):
    b, c = r // 4, r % 4
    toks = _tok_index(c)
    tri, ident = consts_B()
    wrt = np.asarray(inp["moe_w_router"][0], np.float32)
    wr = np.ascontiguousarray(wrt.reshape(DC, 128, NE).transpose(1, 0, 2).reshape(128, DC * NE))
    br = np.ascontiguousarray(np.tile(np.asarray(inp["moe_b_router"][0], np.float32)[None, :], (128, 16)))
    return {
        "HT": outA[r]["HT"], "QT": outA[r]["QT"],
        "KTall": np.stack([outA[4 * b + cc]["KT"] for cc in range(4)]),
        "VVall": np.stack([outA[4 * b + cc]["VV"] for cc in range(4)]),
        "masks": make_masks(c), "tri": tri, "ident": ident,
        "pT": np.ascontiguousarray(inp["p"][1, b, toks, :].T), "vecs": prep_vecs(inp),
        "w_o": inp["b_w_o"][0], "wr": wr, "br": br, "w_egu": inp["moe_w_gu"][0], "w_edn": inp["moe_w_down"][0],
        "w_pg": inp["ple_w_gate"][1], "w_pu": inp["ple_w_up"][1],
    }


def prep_vecs(inp):
    vec_list = [inp["a_norm"][0], inp["a_conv_w"][0, 0], inp["a_conv_w"][0, 1], inp["a_conv_w"][0, 2],
                inp["ffn_norm"][0], inp["ple_norm"][0], inp["kv_norm"], inp["b_norm"][0],
                inp["ffn_norm"][1], inp["ple_norm"][1], inp["final_norm"]]
    return np.ascontiguousarray(np.concatenate([_vec_cols(v) for v in vec_list], axis=1))


def kernel(**inp):
    inp = {k: np.asarray(v) for k, v in inp.items()}
    ncA = build_A()
    resA = run_bass_kernel_spmd(ncA, [prep_A(r, inp) for r in range(8)], core_ids=list(range(8)))
    outA = resA.results
    ncB = build_B()
    resB = run_bass_kernel_spmd(ncB, [prep_B(r, inp, outA) for r in range(8)], core_ids=list(range(8)))
    out = np.zeros((2, 8192, D), np.float32)
    for r in range(8):
        out[r // 4, _tok_index(r % 4), :] = np.asarray(resB.results[r]["YT"]).T
    return out
```
